# Optimizing a Trainium2 kernel written in Bass

```python
import jax, jax.numpy as jnp
from jax import lax
import numpy as np

D_MODEL = 2048
BATCH = 4
SEQ = 4096
DEPTH = 2

GRID_W = 64
CTX_LEN = 256
HEAD_DIM = 128
ROPE_THETA = 10000.0
BLOCK = 128
A_HEADS = 8
A_KV_HEADS = 2
A_GROUP = A_HEADS // A_KV_HEADS
B_HEADS = 8
B_Q_RANK = 512
B_KV_RANK = 256
B_NOPE = 128
B_ROPE = 64
B_VDIM = 128
B_QK = B_NOPE + B_ROPE
C_CH = 1024
CONV_K = 31
D_HEADS = 8
D_KV_HEADS = 2
D_GROUP = D_HEADS // D_KV_HEADS
WINDOW = 128
N_BRANCH = 4
A_W = A_HEADS * HEAD_DIM
A_KV_W = A_KV_HEADS * HEAD_DIM
B_W = B_HEADS * B_VDIM
D_W = D_HEADS * HEAD_DIM
D_KV_W = D_KV_HEADS * HEAD_DIM
OFF_AK = 0
OFF_AV = OFF_AK + A_KV_W
OFF_BCKV = OFF_AV + A_KV_W
OFF_BKR = OFF_BCKV + B_KV_RANK
OFF_DK = OFF_BKR + B_ROPE
OFF_DV = OFF_DK + D_KV_W
KV_COLS = OFF_DV + D_KV_W
OFF_AQ = KV_COLS
OFF_BCQ = OFF_AQ + A_W
OFF_DQ = OFF_BCQ + B_Q_RANK
OFF_GLU = OFF_DQ + D_W
OFF_GATE = OFF_GLU + 2 * C_CH
IN_COLS = OFF_GATE + N_BRANCH * D_MODEL
PEER_HEADS = 8
PEER_NKEYS = 128
PEER_EXPERTS = PEER_NKEYS * PEER_NKEYS
PEER_TOPK = 16
PEER_QDIM = 256
PEER_CHUNK = 128
ALPHA = (2 * DEPTH) ** 0.25
BETA = (8 * DEPTH) ** -0.25
SCALE_HD = HEAD_DIM ** -0.5
SCALE_MLA = B_QK ** -0.5
RMS_EPS = 1e-6
LN_EPS = 1e-5
NEG_INF = -1e30

kernel_name = 'hybrid_gated_branch_diffusion_block'


def _cols(p, off, n):
    return p[..., off:off + n]


def rms_norm(x, g):
    x32 = x.astype(jnp.float32)
    y = x32 * lax.rsqrt(jnp.mean(x32 * x32, axis=-1, keepdims=True) + RMS_EPS)
    return (y * g.astype(jnp.float32)).astype(x.dtype)


def layer_norm(x, g, b):
    x32 = x.astype(jnp.float32)
    mu = jnp.mean(x32, axis=-1, keepdims=True)
    xc = x32 - mu
    y = xc * lax.rsqrt(jnp.mean(xc * xc, axis=-1, keepdims=True) + LN_EPS)
    return (y * g.astype(jnp.float32) + b.astype(jnp.float32)).astype(x.dtype)


def axial_rope_tables(row, col, dim):
    half = dim // 2
    freq = ROPE_THETA ** (-jnp.arange(0, half, 2, dtype=jnp.float32) / half)
    ang = jnp.concatenate([row[:, None] * freq, col[:, None] * freq], axis=-1)
    return jnp.cos(ang), jnp.sin(ang)


def apply_rope(x, cos, sin):
    half = x.shape[-1] // 2
    shape = (1, x.shape[1]) + (1,) * (x.ndim - 3) + (half,)
    cos = cos.reshape(shape).astype(x.dtype)
    sin = sin.reshape(shape).astype(x.dtype)
    x1, x2 = x[..., :half], x[..., half:]
    return jnp.concatenate([x1 * cos - x2 * sin, x2 * cos + x1 * sin], axis=-1)


def kv_parts(p, a_k_norm, b_kv_norm, b_w_kv_up, rope):
    bsz, L = p.shape[:2]
    ka = rms_norm(_cols(p, OFF_AK, A_KV_W).reshape(bsz, L, A_KV_HEADS, HEAD_DIM), a_k_norm)
    va = _cols(p, OFF_AV, A_KV_W).reshape(bsz, L, A_KV_HEADS, HEAD_DIM)
    ckv = rms_norm(_cols(p, OFF_BCKV, B_KV_RANK), b_kv_norm)
    kv_up = (ckv @ b_w_kv_up).reshape(bsz, L, B_HEADS, B_NOPE + B_VDIM)
    kr = _cols(p, OFF_BKR, B_ROPE)
    kd = _cols(p, OFF_DK, D_KV_W).reshape(bsz, L, D_KV_HEADS, HEAD_DIM)
    vd = _cols(p, OFF_DV, D_KV_W).reshape(bsz, L, D_KV_HEADS, HEAD_DIM)
    if rope is not None:
        cos_h, sin_h, cos_r, sin_r = rope
        ka = apply_rope(ka, cos_h, sin_h)
        kr = apply_rope(kr, cos_r, sin_r)
        kd = apply_rope(kd, cos_h, sin_h)
    kb = jnp.concatenate([kv_up[..., :B_NOPE], jnp.broadcast_to(kr[:, :, None, :], (bsz, L, B_HEADS, B_ROPE))], axis=-1)
    vb = kv_up[..., B_NOPE:]
    return ka, va, kb, vb, kd, vd


def q_parts(p, a_q_norm, b_q_norm, b_w_q_up, rope):
    bsz, L = p.shape[:2]
    qa = rms_norm(_cols(p, OFF_AQ, A_W).reshape(bsz, L, A_KV_HEADS, A_GROUP, HEAD_DIM), a_q_norm)
    cq = rms_norm(_cols(p, OFF_BCQ, B_Q_RANK), b_q_norm)
    qb = (cq @ b_w_q_up).reshape(bsz, L, B_HEADS, B_QK)
    qd = _cols(p, OFF_DQ, D_W).reshape(bsz, L, D_KV_HEADS, D_GROUP, HEAD_DIM)
    if rope is not None:
        cos_h, sin_h, cos_r, sin_r = rope
        qa = apply_rope(qa, cos_h, sin_h)
        qb = jnp.concatenate([qb[..., :B_NOPE], apply_rope(qb[..., B_NOPE:], cos_r, sin_r)], axis=-1)
        qd = apply_rope(qd, cos_h, sin_h)
    return qa, qb[:, :, :, None, :], qd


def attend(q, k, v, scale, sink=None, mask=None):
    s = jnp.einsum('bqhgd,bkhd->bhgqk', q, k).astype(jnp.float32) * scale
    if mask is not None:
        s = jnp.where(mask, s, NEG_INF)
    if sink is not None:
        kvh, grp = q.shape[2], q.shape[3]
        sink_col = jnp.broadcast_to(sink.astype(jnp.float32).reshape(1, kvh, grp, 1, 1), s.shape[:-1] + (1,))
        s = jnp.concatenate([s, sink_col], axis=-1)
    pr = jax.nn.softmax(s, axis=-1)
    if sink is not None:
        pr = pr[..., :-1]
    return jnp.einsum('bhgqk,bkhd->bqhgd', pr.astype(v.dtype), v)


def dense_attn(q, k, v, scale):
    bsz, L = q.shape[:2]
    nb = L // BLOCK
    qb = jnp.moveaxis(q.reshape((bsz, nb, BLOCK) + q.shape[2:]), 1, 0)
    o = lax.map(lambda qi: attend(qi, k, v, scale), qb)
    return jnp.moveaxis(o, 0, 1).reshape(bsz, L, -1)


def window_attn(q, k, v, kc, vc, sink, scale):
    bsz, L = q.shape[:2]
    nb = L // BLOCK
    lc = kc.shape[1]
    kp = jnp.pad(k, ((0, 0), (BLOCK, BLOCK), (0, 0), (0, 0)))
    vp = jnp.pad(v, ((0, 0), (BLOCK, BLOCK), (0, 0), (0, 0)))
    qb = jnp.moveaxis(q.reshape((bsz, nb, BLOCK) + q.shape[2:]), 1, 0)
    ctx_ok = jnp.ones((BLOCK, lc), dtype=bool)

    def one_block(args):
        qi, n = args
        start = n * BLOCK
        kw = lax.dynamic_slice_in_dim(kp, start, 3 * BLOCK, axis=1)
        vw = lax.dynamic_slice_in_dim(vp, start, 3 * BLOCK, axis=1)
        qpos = start + jnp.arange(BLOCK)
        kpos = start - BLOCK + jnp.arange(3 * BLOCK)
        band = (jnp.abs(qpos[:, None] - kpos[None, :]) <= WINDOW) & (kpos >= 0)[None, :] & (kpos < L)[None, :]
        mask = jnp.concatenate([ctx_ok, band], axis=1)
        return attend(qi, jnp.concatenate([kc, kw], axis=1), jnp.concatenate([vc, vw], axis=1), scale, sink, mask)

    o = lax.map(one_block, (qb, jnp.arange(nb)))
    return jnp.moveaxis(o, 0, 1).reshape(bsz, L, -1)


def conformer_conv(pglu, conv_w, conv_b, ln_g, ln_b):
    a, gt = jnp.split(pglu, 2, axis=-1)
    u = a * jax.nn.sigmoid(gt)
    y = lax.conv_general_dilated(u, conv_w.reshape(CONV_K, 1, C_CH), window_strides=(1,),
                                 padding=[(CONV_K // 2, CONV_K // 2)],
                                 dimension_numbers=('NWC', 'WIO', 'NWC'),
                                 feature_group_count=C_CH) + conv_b
    return jax.nn.silu(layer_norm(y, ln_g, ln_b))


def merge_branches(branches, pgate, w_brs, w_o):
    gates = pgate.reshape(pgate.shape[:-1] + (N_BRANCH, D_MODEL))
    acc = None
    for n in range(N_BRANCH):
        term = jax.nn.sigmoid(gates[..., n, :]) * (branches[n] @ w_brs[n])
        acc = term if acc is None else acc + term
    return acc @ w_o


def peer_ffn(h, wq, subkeys, u, v):
    bsz, L, dm = h.shape
    half = PEER_QDIM // 2
    n_cand = PEER_TOPK * PEER_TOPK

    def one_chunk(xc):
        q = (xc @ wq).reshape(PEER_CHUNK, PEER_HEADS, 2, half)
        s = jnp.einsum('chpd,hpnd->chpn', q, subkeys).astype(jnp.float32)
        s_top, i_top = lax.top_k(s, PEER_TOPK)
        cand_s = (s_top[:, :, 0, :, None] + s_top[:, :, 1, None, :]).reshape(PEER_CHUNK, PEER_HEADS, n_cand)
        cand_i = (i_top[:, :, 0, :, None] * PEER_NKEYS + i_top[:, :, 1, None, :]).reshape(PEER_CHUNK, PEER_HEADS, n_cand)
        best_s, best_pos = lax.top_k(cand_s, PEER_TOPK)
        idx = jnp.take_along_axis(cand_i, best_pos, axis=-1)
        g = jax.nn.softmax(best_s, axis=-1).astype(xc.dtype)
        act = jax.nn.gelu(jnp.einsum('cd,chkd->chk', xc, u[idx]), approximate=False)
        return jnp.einsum('chk,chkd->cd', g * act, v[idx])

    out = lax.map(one_chunk, h.reshape((bsz * L) // PEER_CHUNK, PEER_CHUNK, dm))
    return out.reshape(bsz, L, dm)


def setup_inputs(seed: int = 0) -> dict:
    key = jax.random.key(seed)
    ks = iter(jax.random.split(key, 40))
    f32 = jnp.float32

    def nrm(shape, scale):
        return jax.random.normal(next(ks), shape, f32) * scale

    def gain(shape):
        return 1.0 + nrm(shape, 0.02)

    L = DEPTH
    return {
        'x': nrm((BATCH, SEQ, D_MODEL), 1.0),
        'c': nrm((BATCH, D_MODEL), 1.0),
        'ctx': nrm((BATCH, CTX_LEN, D_MODEL), 1.0),
        'c_ctx': nrm((D_MODEL,), 1.0),
        'w_ada': nrm((L, D_MODEL, 6 * D_MODEL), 0.5 * D_MODEL ** -0.5),
        'b_ada': nrm((L, 6 * D_MODEL), 0.02),
        'w_in': nrm((L, D_MODEL, IN_COLS), D_MODEL ** -0.5),
        'a_q_norm': gain((L, HEAD_DIM)),
        'a_k_norm': gain((L, HEAD_DIM)),
        'b_q_norm': gain((L, B_Q_RANK)),
        'b_w_q_up': nrm((L, B_Q_RANK, B_HEADS * B_QK), B_Q_RANK ** -0.5),
        'b_kv_norm': gain((L, B_KV_RANK)),
        'b_w_kv_up': nrm((L, B_KV_RANK, B_HEADS * (B_NOPE + B_VDIM)), B_KV_RANK ** -0.5),
        'c_conv_w': nrm((L, CONV_K, C_CH), CONV_K ** -0.5),
        'c_conv_b': nrm((L, C_CH), 0.02),
        'c_ln_g': gain((L, C_CH)),
        'c_ln_b': nrm((L, C_CH), 0.02),
        'd_sink': nrm((L, D_HEADS), 0.5),
        'w_br_a': nrm((L, A_W, D_MODEL), A_W ** -0.5),
        'w_br_b': nrm((L, B_W, D_MODEL), B_W ** -0.5),
        'w_br_c': nrm((L, C_CH, D_MODEL), C_CH ** -0.5),
        'w_br_d': nrm((L, D_W, D_MODEL), D_W ** -0.5),
        'w_o': nrm((L, D_MODEL, D_MODEL), BETA * D_MODEL ** -0.5),
        'ln1_g': gain((L, D_MODEL)),
        'ln1_b': nrm((L, D_MODEL), 0.02),
        'ln2_g': gain((L, D_MODEL)),
        'ln2_b': nrm((L, D_MODEL), 0.02),
        'peer_wq': nrm((L, D_MODEL, PEER_HEADS * PEER_QDIM), D_MODEL ** -0.5),
        'peer_subkeys': nrm((L, PEER_HEADS, 2, PEER_NKEYS, PEER_QDIM // 2), (PEER_QDIM // 2) ** -0.5),
        'peer_u': nrm((L, PEER_EXPERTS, D_MODEL), D_MODEL ** -0.5),
        'peer_v': nrm((L, PEER_EXPERTS, D_MODEL), BETA),
    }


def reference(x, c, ctx, c_ctx, w_ada, b_ada, w_in, a_q_norm, a_k_norm, b_q_norm, b_w_q_up, b_kv_norm, b_w_kv_up,
              c_conv_w, c_conv_b, c_ln_g, c_ln_b, d_sink, w_br_a, w_br_b, w_br_c, w_br_d, w_o,
              ln1_g, ln1_b, ln2_g, ln2_b, peer_wq, peer_subkeys, peer_u, peer_v):
    bsz, S, _ = x.shape
    ROWS = S // GRID_W
    row = jnp.repeat(jnp.arange(ROWS, dtype=jnp.float32), GRID_W)
    col = jnp.tile(jnp.arange(GRID_W, dtype=jnp.float32), ROWS)
    cos_h, sin_h = axial_rope_tables(row, col, HEAD_DIM)
    cos_r, sin_r = axial_rope_tables(row, col, B_ROPE)
    rope_lat = (cos_h, sin_h, cos_r, sin_r)
    sc = jax.nn.silu(c)
    scc = jax.nn.silu(c_ctx)
    xc = ctx
    for l in range(DEPTH):
        last = l == DEPTH - 1
        sh1, s1, g1, sh2, s2, g2 = jnp.split((sc @ w_ada[l] + b_ada[l])[:, None, :], 6, axis=-1)
        csh1, cs1, cg1, csh2, cs2, cg2 = jnp.split(scc @ w_ada[l] + b_ada[l], 6, axis=-1)
        wl = w_in[l]
        w_brs = (w_br_a[l], w_br_b[l], w_br_c[l], w_br_d[l])
        hc = xc * (1 + cs1) + csh1
        pc = hc @ (wl[:, :KV_COLS] if last else wl)
        akc, avc, bkc, bvc, dkc, dvc = kv_parts(pc, a_k_norm[l], b_kv_norm[l], b_w_kv_up[l], None)
        h = x * (1 + s1) + sh1
        p = h @ wl
        ak, av, bk, bv, dk, dv = kv_parts(p, a_k_norm[l], b_kv_norm[l], b_w_kv_up[l], rope_lat)
        qa, qb, qd = q_parts(p, a_q_norm[l], b_q_norm[l], b_w_q_up[l], rope_lat)
        branches = (
            dense_attn(qa, jnp.concatenate([akc, ak], axis=1), jnp.concatenate([avc, av], axis=1), SCALE_HD),
            dense_attn(qb, jnp.concatenate([bkc, bk], axis=1), jnp.concatenate([bvc, bv], axis=1), SCALE_MLA),
            conformer_conv(_cols(p, OFF_GLU, 2 * C_CH), c_conv_w[l], c_conv_b[l], c_ln_g[l], c_ln_b[l]),
            window_attn(qd, dk, dv, dkc, dvc, d_sink[l], SCALE_HD),
        )
        mix = merge_branches(branches, _cols(p, OFF_GATE, N_BRANCH * D_MODEL), w_brs, w_o[l])
        x_new = layer_norm(ALPHA * x + g1 * mix, ln1_g[l], ln1_b[l])
        y = peer_ffn(x_new * (1 + s2) + sh2, peer_wq[l], peer_subkeys[l], peer_u[l], peer_v[l])
        x_new = layer_norm(ALPHA * x_new + g2 * y, ln2_g[l], ln2_b[l])
        if not last:
            lc = xc.shape[1]
            qac, qbc, qdc = q_parts(pc, a_q_norm[l], b_q_norm[l], b_w_q_up[l], None)
            branches_c = (
                attend(qac, akc, avc, SCALE_HD).reshape(bsz, lc, -1),
                attend(qbc, bkc, bvc, SCALE_MLA).reshape(bsz, lc, -1),
                conformer_conv(_cols(pc, OFF_GLU, 2 * C_CH), c_conv_w[l], c_conv_b[l], c_ln_g[l], c_ln_b[l]),
                attend(qdc, dkc, dvc, SCALE_HD, sink=d_sink[l]).reshape(bsz, lc, -1),
            )
            mixc = merge_branches(branches_c, _cols(pc, OFF_GATE, N_BRANCH * D_MODEL), w_brs, w_o[l])
            xc = layer_norm(ALPHA * xc + cg1 * mixc, ln1_g[l], ln1_b[l])
            yc = peer_ffn(xc * (1 + cs2) + csh2, peer_wq[l], peer_subkeys[l], peer_u[l], peer_v[l])
            xc = layer_norm(ALPHA * xc + cg2 * yc, ln2_g[l], ln2_b[l])
        x = x_new
    return x
```

```python
import contextlib
import numpy as np
import concourse.bass as bass
import concourse.mybir as mybir
from concourse.bass_utils import run_bass_kernel_spmd

F32 = mybir.dt.float32
BF16 = mybir.dt.bfloat16
AF = mybir.ActivationFunctionType
ALU = mybir.AluOpType
AX = mybir.AxisListType

D = 2048
DC = 16
CTX = 256
GRID_W = 64
IN_COLS = 14144
ALPHA = 4.0 ** 0.25
SCALE_HD = 128.0 ** -0.5
SCALE_MLA = 192.0 ** -0.5
RMS_EPS = 1e-6
LN_EPS = 1e-5
BIG = 1.0e30


_AMAP = {}


def _region(ap):
    t = ap.tensor
    name = t.name
    es = mybir.dt.size(ap.dtype)
    dims = list(ap.ap)
    off = ap.offset
    if type(t).__name__.startswith('DRam'):
        ext = sum((c - 1) * abs(s) for s, c in dims)
        return (name, 0, 1, off * es, (off + ext + 1) * es)
    space, base = _AMAP[name]
    pstep, pcnt = dims[0]
    if pstep == 0:
        p0, f0 = 0, off
    else:
        p0 = off // pstep
        f0 = off - p0 * pstep
    ext = sum((c - 1) * abs(s) for s, c in dims[1:])
    return (space, p0, p0 + pcnt, base + f0 * es, base + (f0 + ext + 1) * es)


def _overlap(a, b):
    return a[1] < b[2] and b[1] < a[2] and a[3] < b[4] and b[3] < a[4]


def _covers(a, b):
    return a[1] <= b[1] and a[2] >= b[2] and a[3] <= b[3] and a[4] >= b[4]


class Op:
    __slots__ = ('eng', 'fn', 'deps', 'signal', 'sem', 'val', 'is_dma', 'idx', 'barrier')


class Sched:
    ENGS = ('pe', 'act', 'dve', 'pool', 'sp')

    def __init__(self, nc, n_dma_sems=64):
        self.nc = nc
        self.ops = []
        self.recs = {}
        self.n_dma_sems = n_dma_sems
        self.eng_objs = {'pe': nc.tensor, 'act': nc.scalar, 'dve': nc.vector, 'pool': nc.gpsimd, 'sp': nc.sync}

    def add(self, eng, fn, reads=(), writes=(), dma=False, norec=()):
        op = Op()
        op.eng, op.fn, op.is_dma, op.signal = eng, fn, dma, dma
        op.idx = len(self.ops)
        op.sem, op.val, op.barrier = None, 0, False
        deps = set()
        rregs = [_region(a) for a in reads]
        wregs = [_region(a) for a in writes]
        for r in rregs:
            for rec in self.recs.get(r[0], ()):
                if rec[2] and _overlap(rec[0], r):
                    deps.add(rec[1])
        for w in wregs:
            for rec in self.recs.get(w[0], ()):
                if _overlap(rec[0], w):
                    deps.add(rec[1])
        fdeps = []
        for j in deps:
            oj = self.ops[j]
            if (not oj.is_dma) and (not dma) and oj.eng == eng and eng == 'pe':
                continue
            fdeps.append(j)
            oj.signal = True
        op.deps = fdeps
        self.ops.append(op)
        for w in wregs:
            lst = self.recs.setdefault(w[0], [])
            lst[:] = [rec for rec in lst if not _covers(w, rec[0])]
            lst.append([w, op.idx, True])
        nr = [_region(a) for a in norec]
        for r in rregs:
            if r in nr:
                continue
            lst = self.recs.setdefault(r[0], [])
            if not dma:
                lst[:] = [rec for rec in lst if not ((not rec[2]) and rec[0] == r and
                                                     self.ops[rec[1]].eng == eng and not self.ops[rec[1]].is_dma)]
            lst.append([r, op.idx, False])
        return op

    def barrier(self):
        op = Op()
        op.eng, op.fn, op.is_dma, op.signal = None, None, False, False
        op.idx = len(self.ops)
        op.sem, op.val, op.barrier, op.deps = None, 0, True, []
        self.ops.append(op)
        self.recs = {}

    def emit(self, final_wait_ops=()):
        nc = self.nc
        stack = contextlib.ExitStack()
        eng_sems = {e: stack.enter_context(nc.semaphore(f"sem_{e}")) for e in self.ENGS}
        dma_sems = [stack.enter_context(nc.semaphore(f"sem_dma{i}")) for i in range(self.n_dma_sems)]
        last = {}
        for op in self.ops:
            if op.barrier:
                for o in last.values():
                    o.signal = True
            elif not op.is_dma:
                last[op.eng] = op
        cnt = {e: 0 for e in self.ENGS}
        dcnt = [0] * self.n_dma_sems
        dlast = [None] * self.n_dma_sems
        di = 0
        for op in self.ops:
            if op.barrier:
                continue
            if op.is_dma:
                k = di % self.n_dma_sems
                di += 1
                op.sem = ('d', k)
                dcnt[k] += 16
                op.val = dcnt[k]
                if dlast[k] is not None:
                    op.deps = list(set(op.deps) | {dlast[k]})
                dlast[k] = op.idx
            elif op.signal:
                cnt[op.eng] += 1
                op.sem = ('e', op.eng)
                op.val = cnt[op.eng]
        known = {e: {} for e in self.ENGS}
        cur = {}
        nwaits = 0

        def semh(s):
            return dma_sems[s[1]] if s[0] == 'd' else eng_sems[s[1]]

        for op in self.ops:
            if op.barrier:
                for en in self.ENGS:
                    e = self.eng_objs[en]
                    kn = known[en]
                    for s, v in cur.items():
                        if s == ('e', en):
                            continue
                        if kn.get(s, 0) < v:
                            e.wait_ge(semh(s), v)
                            kn[s] = v
                            nwaits += 1
                continue
            e = self.eng_objs[op.eng]
            kn = known[op.eng]
            need = {}
            for j in op.deps:
                oj = self.ops[j]
                if oj.sem is None:
                    continue
                if kn.get(oj.sem, 0) < oj.val:
                    need[oj.sem] = max(need.get(oj.sem, 0), oj.val)
            for s, v in need.items():
                e.wait_ge(semh(s), v)
                kn[s] = v
                nwaits += 1
            ins = op.fn(e)
            if op.signal:
                if op.is_dma:
                    ins.then_inc(dma_sems[op.sem[1]], 16)
                else:
                    ins.then_inc(eng_sems[op.eng], 1)
                cur[op.sem] = op.val
        e = self.eng_objs['sp']
        kn = known['sp']
        for op in final_wait_ops:
            s = op.sem
            if kn.get(s, 0) < op.val:
                e.wait_ge(semh(s), op.val)
                kn[s] = op.val
        self.stats = dict(n_ops=len(self.ops), n_waits=nwaits, cnt=dict(cnt))
        stack.close()


class KB:
    def __init__(self):
        self.nc = bass.Bass("TRN2", target_bir_lowering=False)
        self.S = Sched(self.nc)
        self.uid = 0
        self.top = contextlib.ExitStack()
        self.outs = []
        self.psum = [self.top.enter_context(self.nc.psum_tensor(f"ps{i}", [128, 512], F32)) for i in range(8)]
        for i, p in enumerate(self.psum):
            ml = self.nc.lookup_mls(p).memorylocations[0]
            _AMAP[p.name] = ('PS', ml.bank * 2048 + ml.addr)

    def din(self, name, shape, dt=F32):
        return self.nc.dram_tensor(name, list(shape), dt, kind="ExternalInput").ap()

    def dout(self, name, shape, dt=F32):
        return self.nc.dram_tensor(name, list(shape), dt, kind="ExternalOutput").ap()

    def dscr(self, name, shape, dt=BF16):
        return self.nc.dram_tensor(name, list(shape), dt, kind="Internal").ap()

    def sb(self, st, shape, dt=F32, name='t'):
        self.uid += 1
        t = st.enter_context(self.nc.sbuf_tensor(f"{name}_{self.uid}", list(shape), dt))
        _AMAP[t.name] = ('SB', self.nc.lookup_mls(t).memorylocations[0].addr)
        return t

    def mm(self, out, lhsT, rhs, start=True, stop=True):
        self.S.add('pe', lambda e: e.matmul(out, lhsT=lhsT, rhs=rhs, start=start, stop=stop),
                   reads=[lhsT, rhs], writes=[out])

    def act(self, out, in_, func, bias=None, scale=None, accum_out=None, norec=()):
        reads = [in_]
        kw = {}
        if bias is not None:
            kw['bias'] = bias
            if not isinstance(bias, (int, float)):
                reads.append(bias)
        if scale is not None:
            kw['scale'] = scale
            if not isinstance(scale, (int, float)):
                reads.append(scale)
        writes = [out]
        if accum_out is not None:
            kw['accum_out'] = accum_out
            writes.append(accum_out)
        self.S.add('act', lambda e: e.activation(out=out, in_=in_, func=func, **kw), reads=reads, writes=writes, norec=norec)

    def tt(self, out, in0, in1, op, eng='dve'):
        self.S.add(eng, lambda e: e.tensor_tensor(out=out, in0=in0, in1=in1, op=op), reads=[in0, in1], writes=[out])

    def ts(self, out, in0, s1, s2, op0, op1=None, eng='dve'):
        reads = [in0] + [s for s in (s1, s2) if s is not None and not isinstance(s, (int, float))]
        if op1 is None:
            self.S.add(eng, lambda e: e.tensor_scalar(out=out, in0=in0, scalar1=s1, scalar2=None, op0=op0),
                       reads=reads, writes=[out])
        else:
            self.S.add(eng, lambda e: e.tensor_scalar(out=out, in0=in0, scalar1=s1, scalar2=s2, op0=op0, op1=op1),
                       reads=reads, writes=[out])

    def stt(self, out, in0, scalar, in1, op0, op1, norec=()):
        reads = [in0, in1] + ([] if isinstance(scalar, (int, float)) else [scalar])
        self.S.add('dve', lambda e: e.scalar_tensor_tensor(out=out, in0=in0, scalar=scalar, in1=in1, op0=op0, op1=op1),
                   reads=reads, writes=[out], norec=norec)

    def recip(self, out, in_):
        self.S.add('dve', lambda e: e.reciprocal(out=out, in_=in_), reads=[in_], writes=[out])

    def copy(self, out, in_, eng='dve'):
        if eng == 'act':
            self.act(out, in_, AF.Copy)
        else:
            self.S.add(eng, lambda e: e.tensor_copy(out=out, in_=in_), reads=[in_], writes=[out])

    def memset(self, ap, val, eng='pool'):
        self.S.add(eng, lambda e: e.memset(ap, val), reads=[], writes=[ap])

    def dma(self, out, in_, eng='sp'):
        return self.S.add(eng, lambda e: e.dma_start(out=out, in_=in_), reads=[in_], writes=[out], dma=True)

    def load_cast(self, out, in_):
        return self.dma(out, in_, eng='pool')

    def store(self, out, in_):
        op = self.dma(out, in_)
        self.outs.append(op)
        return op

    def finish(self):
        self.S.emit(final_wait_ops=self.outs)
        self.top.close()
        return self.nc


KVF_AK, KVF_CKV, KVF_KR, KVF_DK = 0, 2, 4, 5
QF_AQ, QF_CQ, QF_DQ, QF_GA, QF_GG, QF_GATE = 0, 8, 12, 20, 28, 36


def build_mixer(S, Tq, first):
    kb = KB()
    nc = kb.nc
    NK = CTX + S
    NKC = NK // 128
    TQC = Tq // 128
    xT = kb.din("xT", [128, DC, S]); xtok = kb.din("xtok", [Tq, D])
    xhalo = kb.din("xhalo", [128, DC, 32]); halomask = kb.din("halomask", [128, 32])
    cT = kb.din("cT", [128, DC, CTX]); ctok = kb.din("ctok", [CTX, D])
    modF = kb.din("modF", [128, 4, DC]); g1bc = kb.din("g1bc", [128, D]); cg1bc = kb.din("cg1bc", [128, D])
    ropeA = kb.din("ropeA", [2, 128, S]); ropeB = kb.din("ropeB", [2, 128, S])
    perms = kb.din("perms", [128, 2, 128])
    WkvF = kb.din("WkvF", [7, 128, DC * 128]); WkvV = kb.din("WkvV", [128, DC * 512]); WqF = kb.din("WqF", [100, 128, DC * 128])
    anorm = kb.din("anorm", [128, 2]); bqn = kb.din("bqn", [128, 4]); bkvn = kb.din("bkvn", [128, 2])
    Wqup = kb.din("Wqup", [128, 12 * 512]); WkvupK = kb.din("WkvupK", [128, 8 * 256]); WkvupV = kb.din("WkvupV", [128, 2 * 1024])
    convw = kb.din("convw", [128, 8, 31]); cvec = kb.din("cvec", [128, 3, 8])
    sink = kb.din("sink", [128, 8]); wmask = kb.din("wmask", [128, 4, 128])
    Wbr = kb.din("Wbr", [16, 128, 4 * 8 * 128]); Wo = kb.din("Wo", [128, DC * D])
    lnbc = kb.din("lnbc", [2, 128, D])
    xmid = kb.dout("xmid", [Tq, D])
    cmid = kb.dout("cmid", [CTX, D])
    KA = kb.dscr("KA", [2, 128, NK]); VA = kb.dscr("VA", [2, NKC, 128, 128])
    KBn = kb.dscr("KBn", [8, 128, NK]); KR = kb.dscr("KR", [128, NK]); VB = kb.dscr("VB", [8, NKC, 128, 128])
    KD = kb.dscr("KD", [2, 128, NK]); VD = kb.dscr("VD", [2, NKC, 128, 128])
    TQA = Tq + (CTX if first else 0)
    QA = kb.dscr("QA", [8, 128, TQA]); QBn = kb.dscr("QBn", [8, 128, TQA]); QBr = kb.dscr("QBr", [4, 128, TQA])
    QD = kb.dscr("QD", [8, 128, TQA])
    UL = 16 + Tq + 16
    U = kb.dscr("U", [8, 128, UL]); UC = kb.dscr("UC", [8, 128, 16 + CTX + 16])
    SG = kb.dscr("SG", [64, 128, TQA])
    BR = kb.dscr("BR", [4, 8, 128, TQA])
    ACC = kb.dscr("ACC", [16, 128, TQA])
    ps = kb.psum

    cst = kb.top
    modS = kb.sb(cst, [128, 4, DC], F32, 'modS')
    kb.dma(modS[:], modF)
    scl = kb.sb(cst, [128, 2, DC], F32, 'scl')
    kb.ts(scl[:, 0, :], modS[:, 0, :], 1.0, None, ALU.add)
    kb.ts(scl[:, 1, :], modS[:, 2, :], 1.0, None, ALU.add)
    permS = kb.sb(cst, [128, 2, 128], BF16, 'perm')
    kb.load_cast(permS[:], perms)
    onesF = kb.sb(cst, [128, 128], F32, 'onesF')
    kb.memset(onesF[:], 1.0)
    onesB = kb.sb(cst, [128, 128], BF16, 'onesB')
    kb.memset(onesB[:], 1.0)
    anormS = kb.sb(cst, [128, 2], F32, 'anorm'); kb.dma(anormS[:], anorm)
    bqnS = kb.sb(cst, [128, 4], F32, 'bqn'); kb.dma(bqnS[:], bqn)
    bkvnS = kb.sb(cst, [128, 2], F32, 'bkvn'); kb.dma(bkvnS[:], bkvn)

    def rope(st, src, cos, sin, perm_idx, N, out_bf):
        sb16 = kb.sb(st, [128, 512], BF16, 'rp16')
        kb.copy(sb16[:, :N], src, eng='act')
        kb.mm(ps[6][:, :N], permS[:, perm_idx, :], sb16[:, :N])
        t1 = kb.sb(st, [128, 512], F32, 'rpt1')
        kb.tt(t1[:, :N], src, cos, ALU.mult, eng='pool')
        t2 = kb.sb(st, [128, 512], F32, 'rpt2')
        kb.tt(t2[:, :N], ps[6][:, :N], sin, ALU.mult)
        kb.tt(out_bf, t1[:, :N], t2[:, :N], ALU.add)

    def rms_stats(st, sq_list, N, inv_n):
        n = len(sq_list)
        for i, sq in enumerate(sq_list):
            kb.mm(ps[7][:, :N], onesF[:], sq, start=(i == 0), stop=(i == n - 1))
        rstd = kb.sb(st, [128, 512], F32, 'rstd')
        kb.act(rstd[:, :N], ps[7][:, :N], AF.Sqrt, bias=RMS_EPS, scale=inv_n)
        kb.recip(rstd[:, :N], rstd[:, :N])
        return rstd

    def proj_block(src_xT, N, mod_i, is_lat, rope_off, kv_pos, q_pos, do_kv, do_q, halo=False):
        with contextlib.ExitStack() as st:
            xs = kb.sb(st, [128, DC, 512], F32, 'xs')
            kb.dma(xs[:, :, :N], src_xT)
            hT = kb.sb(st, [128, DC, 512], BF16, 'hT')
            for k in range(DC):
                kb.ts(hT[:, k, :N], xs[:, k, :N], scl[:, mod_i, k:k + 1], modS[:, 1 + 2 * mod_i, k:k + 1],
                      ALU.mult, ALU.add, eng=('dve' if k % 2 == 0 else 'pool'))
            if is_lat:
                rA = kb.sb(st, [128, 2, 512], F32, 'rA'); rB = kb.sb(st, [128, 2, 512], F32, 'rB')
                for i in range(2):
                    kb.dma(rA[:, i, :N], ropeA[i, :, rope_off:rope_off + N])
                    kb.dma(rB[:, i, :N], ropeB[i, :, rope_off:rope_off + N])
            wbufs = [kb.sb(st, [128, DC, 128], BF16, 'wch') for _ in range(3)]
            wctr = [0]

            def chunk_mm(Wsrc, ci, pbank):
                wb = wbufs[wctr[0] % 3]
                wctr[0] += 1
                kb.load_cast(wb[:], Wsrc[ci].rearrange("p (k c) -> p k c", c=128))
                for k in range(DC):
                    kb.mm(ps[pbank][:, :N], wb[:, k, :], hT[:, k, :N], start=(k == 0), stop=(k == DC - 1))

            pb = [0]

            def nextbank():
                pb[0] = (pb[0] + 1) % 4
                return pb[0]

            def headproc(Wsrc, ci, gain, dst, do_rope, perm_idx, rtab):
                with contextlib.ExitStack() as s2:
                    b = nextbank()
                    chunk_mm(Wsrc, ci, b)
                    cur = kb.sb(s2, [128, 512], F32, 'hp')
                    if gain is not None:
                        sq = kb.sb(s2, [128, 512], F32, 'sq')
                        kb.act(sq[:, :N], ps[b][:, :N], AF.Square)
                        rstd = rms_stats(s2, [sq[:, :N]], N, 1.0 / 128)
                        kb.stt(cur[:, :N], ps[b][:, :N], gain, rstd[:, :N], ALU.mult, ALU.mult)
                    else:
                        kb.copy(cur[:, :N], ps[b][:, :N], eng='act')
                    ob = kb.sb(s2, [128, 512], BF16, 'hpo')
                    if do_rope:
                        rope(s2, cur[:, :N], rtab[:, 0, :N], rtab[:, 1, :N], perm_idx, N, ob[:, :N])
                    else:
                        kb.copy(ob[:, :N], cur[:, :N])
                    kb.dma(dst, ob[:, :N])

            if do_kv:
                for g in range(2):
                    headproc(WkvF, KVF_AK + g, anormS[:, 1:2], KA[g, :, kv_pos:kv_pos + N], is_lat, 0, rA if is_lat else None)
                    headproc(WkvF, KVF_DK + g, None, KD[g, :, kv_pos:kv_pos + N], is_lat, 0, rA if is_lat else None)
                headproc(WkvF, KVF_KR, None, KR[:, kv_pos:kv_pos + N], is_lat, 1, rB if is_lat else None)
                with contextlib.ExitStack() as s2:
                    raw = kb.sb(s2, [128, 2, 512], F32, 'ckvraw'); sq = kb.sb(s2, [128, 2, 512], F32, 'ckvsq')
                    for c in range(2):
                        b = nextbank()
                        chunk_mm(WkvF, KVF_CKV + c, b)
                        kb.copy(raw[:, c, :N], ps[b][:, :N], eng='act')
                        kb.act(sq[:, c, :N], ps[b][:, :N], AF.Square)
                    rstd = rms_stats(s2, [sq[:, 0, :N], sq[:, 1, :N]], N, 1.0 / 256)
                    ckvn = kb.sb(s2, [128, 2, 512], BF16, 'ckvn')
                    for c in range(2):
                        kb.stt(ckvn[:, c, :N], raw[:, c, :N], bkvnS[:, c:c + 1], rstd[:, :N], ALU.mult, ALU.mult)
                    wk = kb.sb(s2, [128, 8, 2, 128], BF16, 'wkvupk'); wv = kb.sb(s2, [128, 2, 1024], BF16, 'wkvupv')
                    kb.load_cast(wk[:], WkvupK.rearrange("p (h c n) -> p h c n", h=8, c=2))
                    kb.load_cast(wv[:], WkvupV.rearrange("p (c n) -> p c n", c=2))
                    for h in range(8):
                        b = nextbank()
                        for c in range(2):
                            kb.mm(ps[b][:, :N], wk[:, h, c, :], ckvn[:, c, :N], start=(c == 0), stop=(c == 1))
                        ob = kb.sb(s2, [128, 512], BF16, 'kbn')
                        kb.copy(ob[:, :N], ps[b][:, :N], eng=('act' if h % 2 else 'dve'))
                        kb.dma(KBn[h, :, kv_pos:kv_pos + N], ob[:, :N])
                    for sub in range(N // 128):
                        vb = kb.sb(s2, [128, 1024], BF16, 'vbt')
                        for half in range(2):
                            b = nextbank()
                            for c in range(2):
                                kb.mm(ps[b][:, :], ckvn[:, c, sub * 128:(sub + 1) * 128], wv[:, c, half * 512:(half + 1) * 512],
                                      start=(c == 0), stop=(c == 1))
                            kb.copy(vb[:, half * 512:(half + 1) * 512], ps[b][:, :], eng=('act' if half else 'dve'))
                        kc = (kv_pos + sub * 128) // 128
                        kb.dma(VB[:, kc, :, :].rearrange("h p d -> p h d"), vb[:].rearrange("p (h d) -> p h d", h=8))
                with contextlib.ExitStack() as s2:
                    wv = kb.sb(s2, [128, DC, 512], BF16, 'wkvv')
                    kb.load_cast(wv[:], WkvV.rearrange("p (k c) -> p k c", c=512))
                    for sub in range(N // 128):
                        b = nextbank()
                        for k in range(DC):
                            kb.mm(ps[b][:, :], hT[:, k, sub * 128:(sub + 1) * 128], wv[:, k, :], start=(k == 0), stop=(k == DC - 1))
                        vt = kb.sb(s2, [128, 512], BF16, 'vt')
                        kb.copy(vt[:], ps[b][:, :], eng='act')
                        kc = (kv_pos + sub * 128) // 128
                        kb.dma(VA[:, kc, :, :].rearrange("g p d -> p g d"), vt[:, 0:256].rearrange("p (g d) -> p g d", g=2))
                        kb.dma(VD[:, kc, :, :].rearrange("g p d -> p g d"), vt[:, 256:512].rearrange("p (g d) -> p g d", g=2))
            if do_q:
                if not halo:
                    for h in range(8):
                        headproc(WqF, QF_AQ + h, anormS[:, 0:1], QA[h, :, q_pos:q_pos + N], is_lat, 0, rA if is_lat else None)
                        headproc(WqF, QF_DQ + h, None, QD[h, :, q_pos:q_pos + N], is_lat, 0, rA if is_lat else None)
                    with contextlib.ExitStack() as s2:
                        raw = kb.sb(s2, [128, 4, 512], F32, 'cqraw'); sq = kb.sb(s2, [128, 4, 512], F32, 'cqsq')
                        for c in range(4):
                            b = nextbank()
                            chunk_mm(WqF, QF_CQ + c, b)
                            kb.copy(raw[:, c, :N], ps[b][:, :N], eng='act')
                            kb.act(sq[:, c, :N], ps[b][:, :N], AF.Square)
                        rstd = rms_stats(s2, [sq[:, c, :N] for c in range(4)], N, 1.0 / 512)
                        cqn = kb.sb(s2, [128, 4, 512], BF16, 'cqn')
                        for c in range(4):
                            kb.stt(cqn[:, c, :N], raw[:, c, :N], bqnS[:, c:c + 1], rstd[:, :N], ALU.mult, ALU.mult)
                        wq = kb.sb(s2, [128, 12, 4, 128], BF16, 'wqup')
                        kb.load_cast(wq[:], Wqup.rearrange("p (j c n) -> p j c n", j=12, c=4))
                        for j in range(12):
                            b = nextbank()
                            for c in range(4):
                                kb.mm(ps[b][:, :N], wq[:, j, c, :], cqn[:, c, :N], start=(c == 0), stop=(c == 3))
                            ob = kb.sb(s2, [128, 512], BF16, 'qbo')
                            if j < 8:
                                kb.copy(ob[:, :N], ps[b][:, :N], eng=('act' if j % 2 else 'dve'))
                                kb.dma(QBn[j, :, q_pos:q_pos + N], ob[:, :N])
                            else:
                                if is_lat:
                                    cur = kb.sb(s2, [128, 512], F32, 'qbr')
                                    kb.copy(cur[:, :N], ps[b][:, :N], eng='act')
                                    rope(s2, cur[:, :N], rB[:, 0, :N], rB[:, 1, :N], 1, N, ob[:, :N])
                                else:
                                    kb.copy(ob[:, :N], ps[b][:, :N], eng='act')
                                kb.dma(QBr[j - 8, :, q_pos:q_pos + N], ob[:, :N])
                    for c in range(64):
                        with contextlib.ExitStack() as s2:
                            b = nextbank()
                            chunk_mm(WqF, QF_GATE + c, b)
                            sg = kb.sb(s2, [128, 512], BF16, 'sg')
                            kb.act(sg[:, :N], ps[b][:, :N], AF.Sigmoid)
                            kb.dma(SG[c, :, q_pos:q_pos + N], sg[:, :N])
                if halo:
                    hm = kb.sb(st, [128, 32], F32, 'hm')
                    kb.dma(hm[:], halomask)
                for j in range(8):
                    with contextlib.ExitStack() as s2:
                        b1 = nextbank(); b2 = nextbank()
                        chunk_mm(WqF, QF_GA + j, b1)
                        chunk_mm(WqF, QF_GG + j, b2)
                        sg = kb.sb(s2, [128, 512], F32, 'glus')
                        kb.act(sg[:, :N], ps[b2][:, :N], AF.Sigmoid)
                        ub = kb.sb(s2, [128, 512], BF16, 'glub')
                        if halo:
                            uf = kb.sb(s2, [128, 32], F32, 'gluf')
                            kb.tt(uf[:, :N], ps[b1][:, :N], sg[:, :N], ALU.mult)
                            kb.tt(ub[:, :N], uf[:, :N], hm[:, :N], ALU.mult)
                            kb.dma(U[j, :, 0:16], ub[:, 0:16])
                            kb.dma(U[j, :, 16 + Tq:32 + Tq], ub[:, 16:32])
                        else:
                            kb.tt(ub[:, :N], ps[b1][:, :N], sg[:, :N], ALU.mult)
                            if is_lat:
                                kb.dma(U[j, :, 16 + q_pos:16 + q_pos + N], ub[:, :N])
                            else:
                                kb.dma(UC[j, :, 16:16 + N], ub[:, :N])

    proj_block(cT, CTX, 1, False, 0, 0, Tq, True, first)
    kb.S.barrier()
    nblk = S // 512
    for bi in range(nblk):
        own = bi * 512 < Tq
        proj_block(xT[:, :, bi * 512:(bi + 1) * 512], 512, 0, True, bi * 512, CTX + bi * 512, bi * 512, True, own)
        kb.S.barrier()
    proj_block(xhalo, 32, 0, True, 0, 0, 0, False, True, halo=True)
    if first:
        with contextlib.ExitStack() as st:
            z = kb.sb(st, [128, 16], BF16, 'zpad')
            kb.memset(z[:], 0.0)
            for j in range(8):
                kb.dma(UC[j, :, 0:16], z[:])
                kb.dma(UC[j, :, 16 + CTX:32 + CTX], z[:])
    kb.S.barrier()

    qblocks = [(bi * 512, 512, list(range(NKC))) for bi in range(Tq // 512)]
    if first:
        qblocks.append((Tq, CTX, [0, 1]))

    def dense_head(st, Kmain, Kex, V, qmain_src, qex_src, scale, dst):
        for qi, (q0, N, chunks) in enumerate(qblocks):
            with contextlib.ExitStack() as s2:
                qm = kb.sb(s2, [128, 512], BF16, 'qm')
                kb.dma(qm[:, :N], qmain_src(q0, N))
                if Kex is not None:
                    qe = kb.sb(s2, [128, 512], BF16, 'qe')
                    p0 = Kex.offset // list(Kex.ap)[0][0]
                    kb.dma(qe[p0:p0 + 64, :N], qex_src(q0, N))
                pts = [kb.sb(s2, [128, 512], BF16, 'pT') for _ in range(3)]
                po, pd = ps[2 + qi % 2], ps[4 + qi % 2]

                def qk(i):
                    kc = chunks[i]
                    b = ps[i % 2]
                    kb.mm(b[:, :N], Kmain[:, kc * 128:(kc + 1) * 128], qm[:, :N], start=True, stop=(Kex is None))
                    if Kex is not None:
                        kb.mm(b[:, :N], Kex[:, kc * 128:(kc + 1) * 128], qe[p0:p0 + 64, :N], start=False, stop=True)

                qk(0)
                nch = len(chunks)
                for i in range(nch):
                    if i + 1 < nch:
                        qk(i + 1)
                    pt = pts[i % 3]
                    kb.act(pt[:, :N], ps[i % 2][:, :N], AF.Exp, scale=scale)
                    kb.mm(po[:, :N], V[:, chunks[i], :], pt[:, :N], start=(i == 0), stop=(i == nch - 1))
                    kb.mm(pd[:, :N], onesB[:], pt[:, :N], start=(i == 0), stop=(i == nch - 1))
                rden = kb.sb(s2, [128, 512], F32, 'rden')
                kb.recip(rden[:, :N], pd[:, :N])
                ob = kb.sb(s2, [128, 512], BF16, 'ao')
                kb.tt(ob[:, :N], po[:, :N], rden[:, :N], ALU.mult)
                kb.dma(dst(q0, N), ob[:, :N])

    for g in range(2):
        with contextlib.ExitStack() as st:
            Ks = kb.sb(st, [128, NK], BF16, 'Ks'); Vs = kb.sb(st, [128, NKC, 128], BF16, 'Vs')
            kb.dma(Ks[:], KA[g]); kb.dma(Vs[:], VA[g].rearrange("c p d -> p c d"))
            for hh in range(4):
                hq = g * 4 + hh
                dense_head(st, Ks, None, Vs, lambda q0, N, hq=hq: QA[hq, :, q0:q0 + N], None, SCALE_HD,
                           lambda q0, N, hq=hq: BR[0, hq, :, q0:q0 + N])
        kb.S.barrier()
    with contextlib.ExitStack() as st0:
        KRs = kb.sb(st0, [128, NK], BF16, 'KRs')
        kb.dma(KRs[:], KR)
        for h in range(8):
            with contextlib.ExitStack() as st:
                Ks = kb.sb(st, [128, NK], BF16, 'Ks'); Vs = kb.sb(st, [128, NKC, 128], BF16, 'Vs')
                kb.dma(Ks[:], KBn[h]); kb.dma(Vs[:], VB[h].rearrange("c p d -> p c d"))
                hp = (h % 2) * 64
                dense_head(st, Ks, KRs[hp:hp + 64, :], Vs, lambda q0, N, h=h: QBn[h, :, q0:q0 + N],
                           lambda q0, N, h=h, hp=hp: QBr[h // 2, hp:hp + 64, q0:q0 + N], SCALE_MLA,
                           lambda q0, N, h=h: BR[1, h, :, q0:q0 + N])
    kb.S.barrier()

    with contextlib.ExitStack() as st0:
        esink = kb.sb(st0, [128, 8], F32, 'esink')
        kb.dma(esink[:], sink)
        kb.act(esink[:], esink[:], AF.Exp)
        wm = kb.sb(st0, [128, 4, 128], BF16, 'wm')
        kb.load_cast(wm[:], wmask)
        nlat = S // 128
        for g in range(2):
            with contextlib.ExitStack() as st:
                Ks = kb.sb(st, [128, NK], BF16, 'Ks'); Vs = kb.sb(st, [128, NKC, 128], BF16, 'Vs')
                kb.dma(Ks[:], KD[g]); kb.dma(Vs[:], VD[g].rearrange("c p d -> p c d"))
                wblocks = []
                for i in range(TQC):
                    Lc = (i - 1) if i > 0 else nlat - 1
                    Rc = i + 1 if i + 1 < nlat else 0
                    wblocks.append((i * 128, [0, 1, 2 + i, 2 + Lc, 2 + Rc], (0 if i > 0 else 2), (1 if i < TQC - 1 else 3)))
                if first:
                    wblocks.append((Tq, [0, 1], None, None))
                    wblocks.append((Tq + 128, [0, 1], None, None))
                for bi, (q0, chunks, mL, mR) in enumerate(wblocks):
                    with contextlib.ExitStack() as s2:
                        q4 = kb.sb(s2, [128, 4, 128], BF16, 'q4')
                        kb.dma(q4[:], QD[g * 4:(g + 1) * 4, :, q0:q0 + 128].rearrange("h p n -> p h n"))
                        nch = len(chunks)
                        po, pd = ps[4 + bi % 2], ps[6 + bi % 2]
                        for hh in range(4):
                            pt = kb.sb(s2, [128, 5, 128], BF16, 'pTw')
                            ba, bb = ps[(hh % 2) * 2], ps[(hh % 2) * 2 + 1]
                            for ci, kc in enumerate(chunks):
                                tgt = ba[:, ci * 128:(ci + 1) * 128] if ci < 4 else bb[:, 0:128]
                                kb.mm(tgt, Ks[:, kc * 128:(kc + 1) * 128], q4[:, hh, :])
                            n1 = min(nch, 4)
                            kb.act(pt[:, 0:n1, :], ba[:, 0:n1 * 128].rearrange("p (c n) -> p c n", n=128), AF.Exp, scale=SCALE_HD)
                            if nch > 4:
                                kb.act(pt[:, 4, :], bb[:, 0:128], AF.Exp, scale=SCALE_HD)
                                kb.tt(pt[:, 3, :], pt[:, 3, :], wm[:, mL, :], ALU.mult, eng='pool')
                                kb.tt(pt[:, 4, :], pt[:, 4, :], wm[:, mR, :], ALU.mult, eng='pool')
                            for ci, kc in enumerate(chunks):
                                kb.mm(po[:, hh * 128:(hh + 1) * 128], Vs[:, kc, :], pt[:, ci, :], start=(ci == 0), stop=(ci == nch - 1))
                            for ci, kc in enumerate(chunks):
                                kb.mm(pd[:, hh * 128:(hh + 1) * 128], onesB[:], pt[:, ci, :], start=(ci == 0), stop=(ci == nch - 1))
                        rden = kb.sb(s2, [128, 4, 128], F32, 'rdenw')
                        for hh in range(4):
                            hq = g * 4 + hh
                            kb.ts(rden[:, hh, :], pd[:, hh * 128:(hh + 1) * 128], esink[:, hq:hq + 1], None, ALU.add)
                        kb.recip(rden[:], rden[:])
                        ob = kb.sb(s2, [128, 4, 128], BF16, 'wo')
                        kb.tt(ob[:], po[:, :].rearrange("p (h n) -> p h n", h=4), rden[:], ALU.mult)
                        kb.dma(BR[3, g * 4:(g + 1) * 4, :, q0:q0 + 128].rearrange("h p n -> p h n"), ob[:])
    kb.S.barrier()

    def conv_branch(Usrc, T, qcol):
        with contextlib.ExitStack() as st:
            cw = kb.sb(st, [128, 8, 31], F32, 'cw'); cv = kb.sb(st, [128, 3, 8], F32, 'cv')
            kb.dma(cw[:], convw); kb.dma(cv[:], cvec)
            Y = kb.sb(st, [128, 8, T], F32, 'Y')
            for j in range(8):
                with contextlib.ExitStack() as s2:
                    ub = kb.sb(s2, [128, T + 32], BF16, 'ub')
                    kb.dma(ub[:], Usrc[j])
                    kb.ts(Y[:, j, :], ub[:, 1:1 + T], cw[:, j, 0:1], cv[:, 0, j:j + 1], ALU.mult, ALU.add)
                    for k in range(1, 31):
                        kb.stt(Y[:, j, :], ub[:, k + 1:k + 1 + T], cw[:, j, k:k + 1], Y[:, j, :], ALU.mult, ALU.add)
            for b0 in range(0, T, 512):
                N = min(512, T - b0)
                with contextlib.ExitStack() as s2:
                    for j in range(8):
                        kb.mm(ps[0][:, :N], onesF[:], Y[:, j, b0:b0 + N], start=(j == 0), stop=(j == 7))
                    for j in range(8):
                        sq = kb.sb(s2, [128, 512], F32, 'csq')
                        kb.act(sq[:, :N], Y[:, j, b0:b0 + N], AF.Square)
                        kb.mm(ps[1][:, :N], onesF[:], sq[:, :N], start=(j == 0), stop=(j == 7))
                    mean = kb.sb(s2, [128, 512], F32, 'cmean'); var = kb.sb(s2, [128, 512], F32, 'cvar')
                    kb.ts(mean[:, :N], ps[0][:, :N], 1.0 / 1024, None, ALU.mult)
                    kb.tt(var[:, :N], mean[:, :N], mean[:, :N], ALU.mult)
                    kb.stt(var[:, :N], ps[1][:, :N], 1.0 / 1024, var[:, :N], ALU.mult, ALU.subtract)
                    kb.act(var[:, :N], var[:, :N], AF.Sqrt, bias=LN_EPS)
                    kb.recip(var[:, :N], var[:, :N])
                    for j in range(8):
                        t = kb.sb(s2, [128, 512], F32, 'ct')
                        kb.tt(t[:, :N], Y[:, j, b0:b0 + N], mean[:, :N], ALU.subtract, eng='pool')
                        kb.tt(t[:, :N], t[:, :N], var[:, :N], ALU.mult)
                        ob = kb.sb(s2, [128, 512], BF16, 'co')
                        kb.act(ob[:, :N], t[:, :N], AF.Silu, bias=cv[:, 2, j:j + 1], scale=cv[:, 1, j:j + 1])
                        kb.dma(BR[2, j, :, qcol + b0:qcol + b0 + N], ob[:, :N])

    conv_branch(U, Tq, 0)
    if first:
        conv_branch(UC, CTX, Tq)
    kb.S.barrier()

    for (q0, N, _) in qblocks:
        with contextlib.ExitStack() as st:
            brt = kb.sb(st, [128, 4, 8, 512], BF16, 'brt')
            for n in range(4):
                kb.dma(brt[:, n, :, :N], BR[n, :, :, q0:q0 + N].rearrange("f p n -> p f n"))
            for m in range(16):
                with contextlib.ExitStack() as s2:
                    wb = kb.sb(s2, [128, 4, 8, 128], BF16, 'wbr')
                    kb.load_cast(wb[:], Wbr[m].rearrange("p (n f c) -> p n f c", n=4, f=8))
                    sg = kb.sb(s2, [128, 4, 512], BF16, 'sgt')
                    kb.dma(sg[:, :, :N], SG[:, :, q0:q0 + N].rearrange("(n m) p t -> m p n t", n=4)[m])
                    acc = kb.sb(s2, [128, 512], F32, 'acc')
                    for n in range(4):
                        b = ps[(m * 4 + n) % 4]
                        for f in range(8):
                            kb.mm(b[:, :N], wb[:, n, f, :], brt[:, n, f, :N], start=(f == 0), stop=(f == 7))
                        if n == 0:
                            kb.tt(acc[:, :N], b[:, :N], sg[:, 0, :N], ALU.mult)
                        else:
                            tmp = kb.sb(s2, [128, 512], F32, 'mtmp')
                            kb.tt(tmp[:, :N], b[:, :N], sg[:, n, :N], ALU.mult)
                            kb.tt(acc[:, :N], acc[:, :N], tmp[:, :N], ALU.add, eng='pool')
                    ab = kb.sb(s2, [128, 512], BF16, 'accb')
                    kb.copy(ab[:, :N], acc[:, :N], eng='act')
                    kb.dma(ACC[m, :, q0:q0 + N], ab[:, :N])
    kb.S.barrier()

    def bn(out, in_, kind):
        if kind == 0:
            kb.S.add('dve', lambda e: e.bn_stats(out=out, in_=in_), reads=[in_], writes=[out])
        else:
            kb.S.add('dve', lambda e: e.bn_aggr(out=out, in_=in_), reads=[in_], writes=[out])

    with contextlib.ExitStack() as st:
        wo = kb.sb(st, [128, DC, D], BF16, 'wo')
        for k in range(DC):
            kb.load_cast(wo[:, k, :], Wo[:, k * D:(k + 1) * D])
        lnS = kb.sb(st, [128, 2, D], F32, 'lnS')
        kb.dma(lnS[:, 0, :], lnbc[0]); kb.dma(lnS[:, 1, :], lnbc[1])
        gS = kb.sb(st, [128, 2, D], F32, 'gS')
        kb.dma(gS[:, 0, :], g1bc)
        if first:
            kb.dma(gS[:, 1, :], cg1bc)
        tiles = [(i * 128, xtok[i * 128:(i + 1) * 128, :], xmid[i * 128:(i + 1) * 128, :], 0) for i in range(TQC)]
        if first:
            tiles += [(Tq + i * 128, ctok[i * 128:(i + 1) * 128, :], cmid[i * 128:(i + 1) * 128, :], 1) for i in range(CTX // 128)]
        for (q0, xsrc, dst, gi) in tiles:
            with contextlib.ExitStack() as s2:
                at = kb.sb(s2, [128, 16, 128], BF16, 'at')
                kb.dma(at[:], ACC[:, :, q0:q0 + 128].rearrange("m p n -> p m n"))
                xt = kb.sb(s2, [128, D], F32, 'xt')
                kb.dma(xt[:], xsrc)
                z = kb.sb(s2, [128, D], F32, 'z')
                stats = kb.sb(s2, [128, 4, 6], F32, 'bst')
                for nb in range(4):
                    b = ps[nb]
                    for k in range(DC):
                        kb.mm(b[:, :], at[:, k, :], wo[:, k, nb * 512:(nb + 1) * 512], start=(k == 0), stop=(k == DC - 1))
                    t = kb.sb(s2, [128, 512], F32, 'wt')
                    kb.tt(t[:], b[:, :], gS[:, gi, nb * 512:(nb + 1) * 512], ALU.mult)
                    kb.stt(z[:, nb * 512:(nb + 1) * 512], xt[:, nb * 512:(nb + 1) * 512], ALPHA, t[:], ALU.mult, ALU.add)
                    bn(stats[:, nb, :], z[:, nb * 512:(nb + 1) * 512], 0)
                mv = kb.sb(s2, [128, 2], F32, 'mv')
                bn(mv[:], stats[:].rearrange("p a b -> p (a b)"), 1)
                rstd = kb.sb(s2, [128, 1], F32, 'lrstd')
                kb.act(rstd[:], mv[:, 1:2], AF.Sqrt, bias=LN_EPS)
                kb.recip(rstd[:], rstd[:])
                kb.ts(z[:], z[:], mv[:, 0:1], rstd[:, 0:1], ALU.subtract, ALU.mult)
                kb.tt(z[:], z[:], lnS[:, 0, :], ALU.mult, eng='pool')
                kb.tt(z[:], z[:], lnS[:, 1, :], ALU.add)
                if gi == 0 or first:
                    kb.store(dst, z[:])
    if not first:
        with contextlib.ExitStack() as st:
            zt = kb.sb(st, [128, D], F32, 'zt')
            kb.memset(zt[:], 0.0)
            for i in range(CTX // 128):
                kb.store(cmid[i * 128:(i + 1) * 128, :], zt[:])
    return kb.finish()


def _chunkT(w):
    K = w.shape[0] // 128
    return np.ascontiguousarray(w.reshape(K, 128, -1).transpose(1, 0, 2))


def _vecP(v):
    return np.ascontiguousarray(v.reshape(-1, 128).T)


def _rope_tables(pos):
    pos = np.asarray(pos)
    row = (pos // GRID_W).astype(np.float64)
    col = (pos % GRID_W).astype(np.float64)

    def tab(dim):
        half = dim // 2
        freq = 10000.0 ** (-np.arange(0, half, 2, dtype=np.float64) / half)
        ang = np.concatenate([row[:, None] * freq, col[:, None] * freq], axis=-1)
        return np.cos(ang).T, np.sin(ang).T
    cA, sA = tab(128)
    cB, sB = tab(64)
    ropeA = np.stack([np.concatenate([cA, cA], 0), np.concatenate([-sA, sA], 0)]).astype(np.float32)
    ropeB = np.stack([np.concatenate([cB, cB, cB, cB], 0), np.concatenate([-sB, sB, -sB, sB], 0)]).astype(np.float32)
    return ropeA, ropeB


def _perms():
    P = np.zeros((128, 2, 128), np.float32)
    for m in range(128):
        P[(m + 64) % 128, 0, m] = 1.0
        P[64 * (m // 64) + ((m % 64) + 32) % 64, 1, m] = 1.0
    return P


def pack_mixer_weights(inp, l):
    w = inp['w_in'][l]
    c = lambda a, b: _chunkT(w[:, a:b]).reshape(128, -1)
    kvF = [c(0, 128), c(128, 256), c(512, 640), c(640, 768),
           _chunkT(np.concatenate([w[:, 768:832], w[:, 768:832]], 1)).reshape(128, -1), c(832, 960), c(960, 1088)]
    kvV = _chunkT(np.concatenate([w[:, 256:512], w[:, 1088:1344]], 1)).reshape(128, -1)
    qcols = [1344 + 128 * i for i in range(8)] + [2368 + 128 * i for i in range(4)] + [2880 + 128 * i for i in range(8)] + \
            [3904 + 128 * i for i in range(8)] + [4928 + 128 * i for i in range(8)] + [5952 + 128 * i for i in range(64)]
    qF = np.stack([c(a, a + 128) for a in qcols])
    qu = inp['b_w_q_up'][l]
    qch = [qu[:, h * 192:h * 192 + 128] for h in range(8)] + \
          [np.concatenate([qu[:, (2 * j) * 192 + 128:(2 * j) * 192 + 192], qu[:, (2 * j + 1) * 192 + 128:(2 * j + 1) * 192 + 192]], 1) for j in range(4)]
    Wqup = np.stack([_chunkT(x) for x in qch], 1).reshape(128, -1)
    ku = inp['b_w_kv_up'][l]
    WkvupK = np.stack([_chunkT(ku[:, h * 256:h * 256 + 128]) for h in range(8)], 1).reshape(128, -1)
    WkvupV = _chunkT(np.concatenate([ku[:, h * 256 + 128:h * 256 + 256] for h in range(8)], 1)).reshape(128, -1)
    brs = [inp[k][l] for k in ('w_br_a', 'w_br_b', 'w_br_c', 'w_br_d')]
    Wbr = np.stack([np.stack([_chunkT(b[:, m * 128:(m + 1) * 128]) for b in brs], 1) for m in range(16)]).reshape(16, 128, -1)
    d = dict(
        WkvF=np.stack(kvF), WkvV=kvV, WqF=qF,
        anorm=np.stack([inp['a_q_norm'][l], inp['a_k_norm'][l]], 1),
        bqn=_vecP(inp['b_q_norm'][l]), bkvn=_vecP(inp['b_kv_norm'][l]),
        Wqup=Wqup, WkvupK=WkvupK, WkvupV=WkvupV,
        convw=np.ascontiguousarray(inp['c_conv_w'][l].reshape(31, 8, 128).transpose(2, 1, 0)),
        cvec=np.stack([_vecP(inp['c_conv_b'][l]), _vecP(inp['c_ln_g'][l]), _vecP(inp['c_ln_b'][l])], 1),
        sink=np.broadcast_to(inp['d_sink'][l][None, :], (128, 8)),
        Wbr=Wbr, Wo=_chunkT(inp['w_o'][l]).reshape(128, -1),
        lnbc=np.stack([np.broadcast_to(inp['ln1_g'][l][None, :], (128, D)), np.broadcast_to(inp['ln1_b'][l][None, :], (128, D))]),
        perms=_perms(),
    )
    return {k: np.ascontiguousarray(v, dtype=np.float32) for k, v in d.items()}


def pack_mixer_core(xb, xcb, mod_b, cmod, S, Tq, half):
    nh = S // Tq
    own = np.arange(half * Tq, (half + 1) * Tq)
    other = np.concatenate([np.arange(((half + 1 + i) % nh) * Tq, ((half + 1 + i) % nh + 1) * Tq) for i in range(nh - 1)]) \
        if nh > 1 else np.zeros((0,), np.int64)
    order = np.concatenate([own, other]).astype(np.int64)
    xl = xb[order]
    ropeA, ropeB = _rope_tables(order)
    hpos = np.concatenate([np.arange(half * Tq - 16, half * Tq), np.arange((half + 1) * Tq, (half + 1) * Tq + 16)])
    valid = (hpos >= 0) & (hpos < S)
    xh = np.zeros((32, D), np.float32)
    xh[valid] = xb[hpos[valid]]
    rAh, rBh = _rope_tables(np.where(valid, hpos, 0))
    tri = np.tril(np.ones((128, 128), np.float32))
    L, Rm = tri, tri.T
    zero = np.zeros_like(tri)
    wmask = np.stack([L, Rm, L if half > 0 else zero, Rm if half < nh - 1 else zero], 1)
    sh1, s1, g1 = mod_b[0:D], mod_b[D:2 * D], mod_b[2 * D:3 * D]
    csh1, cs1, cg1 = cmod[0:D], cmod[D:2 * D], cmod[2 * D:3 * D]
    d = dict(
        xT=_chunkT(xl.T.copy()) if False else np.ascontiguousarray(xl.reshape(S, DC, 128).transpose(2, 1, 0)),
        xtok=xb[own], xhalo=np.ascontiguousarray(xh.reshape(32, DC, 128).transpose(2, 1, 0)),
        halomask=np.broadcast_to(valid.astype(np.float32)[None, :], (128, 32)),
        cT=np.ascontiguousarray(xcb.reshape(CTX, DC, 128).transpose(2, 1, 0)), ctok=xcb,
        modF=np.stack([_vecP(s1), _vecP(sh1), _vecP(cs1), _vecP(csh1)], 1),
        g1bc=np.broadcast_to(g1[None, :], (128, D)), cg1bc=np.broadcast_to(cg1[None, :], (128, D)),
        ropeA=ropeA, ropeB=ropeB, wmask=wmask,
    )
    return {k: np.ascontiguousarray(v, dtype=np.float32) for k, v in d.items()}


def build_peer(blocks, Tp):
    kb = KB()
    ps = kb.psum
    hTd = kb.din("hT", [128, DC, Tp]); xtok = kb.din("xtok", [Tp, D])
    pmodF = kb.din("pmodF", [128, 2, 2, DC]); g2bc = kb.din("g2bc", [2, 128, D])
    Wq = kb.din("Wq", [16, 128, DC * 128]); SK = kb.din("SK", [128, 16 * 128])
    UT = kb.din("UT", [128, 128, DC * 128]); V = kb.din("V", [16384, D])
    identd = kb.din("ident", [128, 128]); ln2bc = kb.din("ln2bc", [2, 128, D])
    yout = kb.dout("yout", [Tp, D])
    cst = kb.top
    pm = kb.sb(cst, [128, 2, 2, DC], F32, 'pm')
    kb.dma(pm[:], pmodF)
    for sset in range(2):
        kb.ts(pm[:, sset, 0, :], pm[:, sset, 0, :], 1.0, None, ALU.add)
    identB = kb.sb(cst, [128, 128], BF16, 'identB')
    kb.load_cast(identB[:], identd)
    skS = kb.sb(cst, [128, 16, 128], BF16, 'skS')
    kb.load_cast(skS[:], SK.rearrange("p (c n) -> p c n", c=16))
    GRP = 4

    def dmax(o, i):
        kb.S.add('dve', lambda e: e.max(out=o, in_=i), reads=[i], writes=[o])

    def dmr(o, m8, i):
        kb.S.add('dve', lambda e: e.match_replace(out=o, in_to_replace=m8, in_values=i, imm_value=-BIG),
                 reads=[m8, i], writes=[o])

    def bn(out, in_, kind):
        if kind == 0:
            kb.S.add('dve', lambda e: e.bn_stats(out=out, in_=in_), reads=[in_], writes=[out])
        else:
            kb.S.add('dve', lambda e: e.bn_aggr(out=out, in_=in_), reads=[in_], writes=[out])

    for (t0, N, mset) in blocks:
        nt = N // 128
        with contextlib.ExitStack() as st:
            h2T = kb.sb(st, [128, DC, 512], BF16, 'h2T')
            At = kb.sb(st, [128, 4, 8, 128], F32, 'At'); Bt = kb.sb(st, [128, 4, 8, 128], F32, 'Bt')
            s1m = kb.sb(st, [128, 4, 8, 128], F32, 's1m')
            Yacc = kb.sb(st, [128, 4, D], F32, 'Yacc')
            with contextlib.ExitStack() as s2:
                xs = kb.sb(s2, [128, DC, 512], F32, 'pxs')
                kb.dma(xs[:, :, :N], hTd[:, :, t0:t0 + N])
                for k in range(DC):
                    kb.ts(h2T[:, k, :N], xs[:, k, :N], pm[:, mset, 0, k:k + 1], pm[:, mset, 1, k:k + 1], ALU.mult, ALU.add,
                          eng=('dve' if k % 2 == 0 else 'pool'))
                qT = kb.sb(s2, [128, 16, 512], BF16, 'qT')
                wbufs = [kb.sb(s2, [128, DC, 128], BF16, 'wqc') for _ in range(3)]
                for c in range(16):
                    wb = wbufs[c % 3]
                    kb.load_cast(wb[:], Wq[c].rearrange("p (k n) -> p k n", n=128))
                    b = ps[c % 2]
                    for k in range(DC):
                        kb.mm(b[:, :N], wb[:, k, :], h2T[:, k, :N], start=(k == 0), stop=(k == DC - 1))
                    kb.copy(qT[:, c, :N], b[:, :N], eng=('act' if c % 2 else 'dve'))
                for t in range(nt):
                    with contextlib.ExitStack() as s3:
                        sc = kb.sb(s3, [128, 16, 128], F32, 'sc')
                        for c4 in range(4):
                            b = ps[2 + c4 % 2]
                            for cc in range(4):
                                c = c4 * 4 + cc
                                kb.mm(b[:, cc * 128:(cc + 1) * 128], qT[:, c, t * 128:(t + 1) * 128], skS[:, c, :])
                            kb.copy(sc[:, c4 * 4:(c4 + 1) * 4, :], b[:, :].rearrange("p (c n) -> p c n", c=4), eng='act')
                        top = kb.sb(s3, [128, 16, 16], F32, 'top')
                        scr = kb.sb(s3, [128, 128], F32, 'scr')
                        for c in range(16):
                            dmax(top[:, c, 0:8], sc[:, c, :])
                            dmr(scr[:], top[:, c, 0:8], sc[:, c, :])
                            dmax(top[:, c, 8:16], scr[:])
                        for h in range(8):
                            c0, c1 = 2 * h, 2 * h + 1
                            cand = kb.sb(s3, [128, 16, 16], F32, 'cand')
                            kb.tt(cand[:], top[:, c0, :].unsqueeze(2).broadcast_to([128, 16, 16]),
                                  top[:, c1, :].unsqueeze(1).broadcast_to([128, 16, 16]), ALU.add)
                            cf = cand[:].rearrange("p a b -> p (a b)")
                            b24 = kb.sb(s3, [128, 24], F32, 'b24')
                            c2 = kb.sb(s3, [128, 256], F32, 'cand2')
                            dmax(b24[:, 0:8], cf)
                            dmr(c2[:], b24[:, 0:8], cf)
                            dmax(b24[:, 8:16], c2[:])
                            dmr(c2[:], b24[:, 8:16], c2[:])
                            dmax(b24[:, 16:24], c2[:])
                            sm = kb.sb(s3, [128, 8], F32, 'sm')
                            kb.ts(sm[:, 0:1], b24[:, 15:16], b24[:, 16:17], 0.5, ALU.add, ALU.mult)
                            kb.ts(sm[:, 1:2], b24[:, 0:1], -1.0, None, ALU.mult)
                            junk = kb.sb(s3, [128, 16], F32, 'junk')
                            kb.act(junk[:], b24[:, 0:16], AF.Exp, bias=sm[:, 1:2], accum_out=sm[:, 2:3])
                            kb.act(sm[:, 4:5], sm[:, 2:3], AF.Ln)
                            kb.ts(sm[:, 3:4], sm[:, 4:5], -1.0, sm[:, 1:2], ALU.mult, ALU.add)
                            kb.ts(Bt[:, t, h, :], sc[:, c0, :], sm[:, 3:4], None, ALU.add)
                            ta = kb.sb(s3, [128, 128], F32, 'ta'); tb = kb.sb(s3, [128, 128], F32, 'tb')
                            kb.ts(ta[:], sc[:, c0, :], -1.0, sm[:, 0:1], ALU.mult, ALU.add)
                            kb.ts(tb[:], sc[:, c0, :], top[:, c0, 15:16], BIG, ALU.is_lt, ALU.mult)
                            kb.tt(At[:, t, h, :], ta[:], tb[:], ALU.add, eng='pool')
                            tc_ = kb.sb(s3, [128, 128], F32, 'tc')
                            kb.ts(tc_[:], sc[:, c1, :], top[:, c1, 15:16], -BIG, ALU.is_lt, ALU.mult)
                            kb.tt(s1m[:, t, h, :], sc[:, c1, :], tc_[:], ALU.add, eng='pool')
            with contextlib.ExitStack() as s2:
                ubufs = [kb.sb(s2, [128, DC, 128], BF16, 'uT') for _ in range(3)]
                vbufs = [kb.sb(s2, [128, GRP, D], BF16, 'vch') for _ in range(2)]
                gbufs = [kb.sb(s2, [128, GRP, 512], BF16, 'gT') for _ in range(2)]
                wbs = [kb.sb(s2, [128, 128], BF16, 'wexp') for _ in range(6)]
                cbs = [kb.sb(s2, [128, 128], BF16, 'cmask') for _ in range(6)]
                gels = [kb.sb(s2, [128, 512], F32, 'gel') for _ in range(2)]
                ctr = 0
                for i0 in range(128):
                    grp, j = divmod(i0, GRP)
                    ub = ubufs[i0 % 3]
                    kb.load_cast(ub[:], UT[i0].rearrange("p (k e) -> p k e", e=128))
                    vb = vbufs[grp % 2]
                    kb.load_cast(vb[:, j, :], V[i0 * 128:(i0 + 1) * 128, :])
                    pa = ps[i0 % 2]
                    for k in range(DC):
                        kb.mm(pa[:, :N], ub[:, k, :], h2T[:, k, :N], start=(k == 0), stop=(k == DC - 1))
                    gel = gels[i0 % 2]
                    kb.act(gel[:, :N], pa[:, :N], AF.Gelu)
                    pc = ps[2 + i0 % 2]
                    for t in range(nt):
                        for h in range(8):
                            wbuf = wbs[ctr % 6]; cbuf = cbs[ctr % 6]; ctr += 1
                            kb.act(wbuf[:], s1m[:, t, h, :], AF.Exp, bias=Bt[:, t, h, i0:i0 + 1], norec=[Bt[:, t, h, i0:i0 + 1]])
                            kb.stt(cbuf[:], s1m[:, t, h, :], At[:, t, h, i0:i0 + 1], wbuf[:], ALU.is_ge, ALU.mult, norec=[At[:, t, h, i0:i0 + 1]])
                            kb.mm(pc[:, t * 128:(t + 1) * 128], cbuf[:], identB[:], start=(h == 0), stop=(h == 7))
                    gb = gbufs[grp % 2]
                    kb.tt(gb[:, j, :N], gel[:, :N], pc[:, :N], ALU.mult)
                    if j == GRP - 1:
                        for t in range(nt):
                            for nb in range(4):
                                po = ps[4 + (t * 4 + nb) % 4]
                                for jj in range(GRP):
                                    kb.mm(po[:, :], gb[:, jj, t * 128:(t + 1) * 128], vb[:, jj, nb * 512:(nb + 1) * 512],
                                          start=(jj == 0), stop=(jj == GRP - 1))
                                dst = Yacc[:, t, nb * 512:(nb + 1) * 512]
                                if grp == 0:
                                    kb.copy(dst, po[:, :], eng='act')
                                else:
                                    kb.tt(dst, dst, po[:, :], ALU.add, eng='dve')
            with contextlib.ExitStack() as s2:
                lnS = kb.sb(s2, [128, 2, D], F32, 'ln2S')
                kb.dma(lnS[:, 0, :], ln2bc[0]); kb.dma(lnS[:, 1, :], ln2bc[1])
                gS = kb.sb(s2, [128, D], F32, 'g2S')
                kb.dma(gS[:], g2bc[mset])
                for t in range(nt):
                    with contextlib.ExitStack() as s3:
                        xt = kb.sb(s3, [128, D], F32, 'pxt')
                        kb.dma(xt[:], xtok[t0 + t * 128:t0 + (t + 1) * 128, :])
                        z = kb.sb(s3, [128, D], F32, 'pz')
                        kb.tt(z[:], Yacc[:, t, :], gS[:], ALU.mult, eng='pool')
                        kb.stt(z[:], xt[:], ALPHA, z[:], ALU.mult, ALU.add)
                        stats = kb.sb(s3, [128, 4, 6], F32, 'pbst')
                        for nb in range(4):
                            bn(stats[:, nb, :], z[:, nb * 512:(nb + 1) * 512], 0)
                        mv = kb.sb(s3, [128, 2], F32, 'pmv')
                        bn(mv[:], stats[:].rearrange("p a b -> p (a b)"), 1)
                        rstd = kb.sb(s3, [128, 1], F32, 'prstd')
                        kb.act(rstd[:], mv[:, 1:2], AF.Sqrt, bias=LN_EPS)
                        kb.recip(rstd[:], rstd[:])
                        kb.ts(z[:], z[:], mv[:, 0:1], rstd[:, 0:1], ALU.subtract, ALU.mult)
                        kb.tt(z[:], z[:], lnS[:, 0, :], ALU.mult, eng='pool')
                        kb.tt(z[:], z[:], lnS[:, 1, :], ALU.add)
                        kb.store(yout[t0 + t * 128:t0 + (t + 1) * 128, :], z[:])
        kb.S.barrier()
    return kb.finish()


def pack_peer_weights(inp, l):
    wq = inp['peer_wq'][l]
    sk = inp['peer_subkeys'][l]
    u = inp['peer_u'][l]
    d = dict(
        Wq=np.stack([_chunkT(wq[:, c * 128:(c + 1) * 128]).reshape(128, -1) for c in range(16)]),
        SK=np.ascontiguousarray(sk.reshape(16, 128, 128).transpose(2, 0, 1)).reshape(128, -1),
        UT=np.ascontiguousarray(u.reshape(128, 128, DC, 128).transpose(0, 3, 2, 1)).reshape(128, 128, -1),
        V=inp['peer_v'][l],
        ident=np.eye(128, dtype=np.float32),
        ln2bc=np.stack([np.broadcast_to(inp['ln2_g'][l][None, :], (128, D)), np.broadcast_to(inp['ln2_b'][l][None, :], (128, D))]),
    )
    return {k: np.ascontiguousarray(v, dtype=np.float32) for k, v in d.items()}


def pack_peer_core(xtoks, mod_b, cmod):
    Tp = xtoks.shape[0]
    sh2, s2, g2 = mod_b[3 * D:4 * D], mod_b[4 * D:5 * D], mod_b[5 * D:6 * D]
    csh2, cs2, cg2 = cmod[3 * D:4 * D], cmod[4 * D:5 * D], cmod[5 * D:6 * D]
    d = dict(
        hT=np.ascontiguousarray(xtoks.reshape(Tp, DC, 128).transpose(2, 1, 0)), xtok=xtoks,
        pmodF=np.stack([np.stack([_vecP(s2), _vecP(sh2)], 1), np.stack([_vecP(cs2), _vecP(csh2)], 1)], 1),
        g2bc=np.stack([np.broadcast_to(g2[None, :], (128, D)), np.broadcast_to(cg2[None, :], (128, D))]),
    )
    return {k: np.ascontiguousarray(v, dtype=np.float32) for k, v in d.items()}


MODC = 6 * D // 8


def build_mod():
    kb = KB()
    ps = kb.psum
    cv = kb.din("cv", [128, DC, 8]); Wa = kb.din("Wa", [2, 128, DC, MODC]); ba = kb.din("ba", [2, 8, MODC])
    mo = kb.dout("mod", [2, 8, MODC])
    st = kb.top
    c = kb.sb(st, [128, DC, 8], F32, 'cvs')
    kb.dma(c[:], cv)
    sc = kb.sb(st, [128, DC, 8], F32, 'scs')
    kb.act(sc[:], c[:], AF.Silu)
    for l in range(2):
        for nb in range(MODC // 512):
            with contextlib.ExitStack() as s2:
                w = kb.sb(s2, [128, DC, 512], F32, 'was')
                kb.dma(w[:], Wa[l, :, :, nb * 512:(nb + 1) * 512])
                b = kb.sb(s2, [8, 512], F32, 'bas')
                kb.dma(b[:], ba[l, :, nb * 512:(nb + 1) * 512])
                p = ps[nb % 2]
                for k in range(DC):
                    kb.mm(p[0:8, :], sc[:, k, :], w[:, k, :], start=(k == 0), stop=(k == DC - 1))
                o = kb.sb(s2, [8, 512], F32, 'mos')
                kb.tt(o[:], p[0:8, :], b[:], ALU.add)
                kb.store(mo[l, :, nb * 512:(nb + 1) * 512], o[:])
    return kb.finish()


_NC_CACHE = {}


def _get(key, fn):
    if key not in _NC_CACHE:
        _NC_CACHE[key] = fn()
    return _NC_CACHE[key]


def kernel(**inputs):
    inp = {k: np.asarray(v, dtype=np.float32) for k, v in inputs.items()}
    B, S, _ = inp['x'].shape
    NCORE = 8
    nh = NCORE // B
    Tq = S // nh
    cores = list(range(NCORE))
    rows = np.zeros((8, D), np.float32)
    rows[:B] = inp['c']
    rows[B] = inp['c_ctx']
    cv = np.ascontiguousarray(rows.reshape(8, DC, 128).transpose(2, 1, 0))
    maps = []
    for c in cores:
        cs = slice(c * MODC, (c + 1) * MODC)
        maps.append(dict(cv=cv,
                         Wa=np.stack([_chunkT(inp['w_ada'][l][:, cs]) for l in range(2)]),
                         ba=np.stack([np.broadcast_to(inp['b_ada'][l][None, cs], (8, MODC)) for l in range(2)]).astype(np.float32)))
    res = run_bass_kernel_spmd(_get('mod', build_mod), maps, core_ids=cores)
    mods = np.concatenate([r['mod'] for r in res.results], axis=2)
    x = inp['x']
    xc = inp['ctx']
    for l in range(2):
        first = l == 0
        W = pack_mixer_weights(inp, l)
        maps = []
        for c in cores:
            b, half = c // nh, c % nh
            m = dict(W)
            m.update(pack_mixer_core(x[b], xc[b], mods[l, b], mods[l, B], S, Tq, half))
            maps.append(m)
        res = run_bass_kernel_spmd(_get(('mixer', S, Tq, first), lambda: build_mixer(S, Tq, first)), maps, core_ids=cores)
        del maps, W
        xmid = np.stack([np.concatenate([res.results[b * nh + h]['xmid'] for h in range(nh)], 0) for b in range(B)])
        cmid = np.stack([res.results[b * nh]['cmid'] for b in range(B)])
        Wp = pack_peer_weights(inp, l)
        cper = CTX // nh
        blocks = [(i * 512, 512, 0) for i in range(Tq // 512)]
        Tp = Tq
        if first:
            blocks.append((Tq, cper, 1))
            Tp = Tq + cper
        maps = []
        for c in cores:
            b, half = c // nh, c % nh
            toks = xmid[b, half * Tq:(half + 1) * Tq]
            if first:
                toks = np.concatenate([toks, cmid[b, half * cper:(half + 1) * cper]], 0)
            m = dict(Wp)
            m.update(pack_peer_core(toks, mods[l, b], mods[l, B]))
            maps.append(m)
        res = run_bass_kernel_spmd(_get(('peer', Tp, first), lambda: build_peer(blocks, Tp)), maps, core_ids=cores)
        del maps, Wp
        x = np.stack([np.concatenate([res.results[b * nh + h]['yout'][:Tq] for h in range(nh)], 0) for b in range(B)])
        if first:
            xc = np.stack([np.concatenate([res.results[b * nh + h]['yout'][Tq:] for h in range(nh)], 0) for b in range(B)])
    return np.ascontiguousarray(x, dtype=np.float32)
```
